# Optimizing a Trainium2 kernel written in Bass

```python
import jax, jax.numpy as jnp
from jax import lax
import numpy as np

D_MODEL = 1024
BATCH = 4
SEQ = 4096
DEPTH = 4

ROPE_THETA = 500000.0
ROPE_DIM = 16
Q_BLOCK = 128
NORM_EPS = 1e-6

DSA_HEADS = 8
DSA_HEAD_DIM = 64
DSA_NOPE_DIM = DSA_HEAD_DIM - ROPE_DIM
DSA_KV_RANK = 128
DSA_V_DIM = 64
IDX_HEADS = 4
IDX_DIM = 64
INDEX_TOPK = 256

SB_HEADS = 8
SB_HEAD_DIM = 64

ML_HEADS = 4
ML_HEAD_DIM = 128
ML_CHUNK = 64
ML_CONV = 4

N_BRANCHES = 3
BRANCH_WIDTH = 512
DSA_Q_WIDTH = DSA_HEADS * DSA_HEAD_DIM
SB_WIDTH = SB_HEADS * SB_HEAD_DIM
ML_WIDTH = ML_HEADS * ML_HEAD_DIM

IN_WIDTHS = (
    DSA_Q_WIDTH,
    DSA_KV_RANK,
    ROPE_DIM,
    IDX_HEADS * IDX_DIM,
    IDX_DIM,
    IDX_HEADS,
    BRANCH_WIDTH,
    SB_WIDTH,
    SB_WIDTH,
    SB_WIDTH,
    BRANCH_WIDTH,
    2 * ML_WIDTH,
    ML_WIDTH,
    ML_HEADS,
    ML_HEADS,
    ML_WIDTH,
    BRANCH_WIDTH,
    N_BRANCHES * D_MODEL,
)
W_IN_COLS = sum(IN_WIDTHS)

kernel_name = 'hybrid_dsa_stickbreak_mlstm_trunk'


def rms_norm(x, w):
    x32 = x.astype(jnp.float32)
    y = x32 * lax.rsqrt(jnp.mean(x32 * x32, axis=-1, keepdims=True) + NORM_EPS)
    return (y * w.astype(jnp.float32)).astype(x.dtype)


def rope_tables(seq):
    pos = jnp.arange(seq, dtype=jnp.float32)
    inv = ROPE_THETA ** (-jnp.arange(0, ROPE_DIM, 2, dtype=jnp.float32) / ROPE_DIM)
    ang = pos[:, None] * inv[None, :]
    return jnp.cos(ang), jnp.sin(ang)


def partial_rope(x, cos, sin):
    half = ROPE_DIM // 2
    x1 = x[..., :half]
    x2 = x[..., half:ROPE_DIM]
    out = jnp.concatenate([x1 * cos - x2 * sin, x2 * cos + x1 * sin, x[..., ROPE_DIM:]], axis=-1)
    return out.astype(x.dtype)


def to_blocks(a):
    b, s = a.shape[:2]
    a = a.reshape((b, s // Q_BLOCK, Q_BLOCK) + a.shape[2:])
    return jnp.moveaxis(a, 1, 0)


def from_blocks(a):
    a = jnp.moveaxis(a, 0, 1)
    return a.reshape((a.shape[0], a.shape[1] * a.shape[2]) + a.shape[3:])


def dsa_attention(q, c_kv, k_rope, iq, ik, iw, w_uk, w_uv, kv_norm_w, cos, sin):
    bsz, seq = q.shape[:2]
    topk = min(INDEX_TOPK, seq // 4)
    c_kv = rms_norm(c_kv, kv_norm_w)
    q_rope = partial_rope(q[..., :ROPE_DIM], cos[:, None], sin[:, None])
    q_nope = q[..., ROPE_DIM:]
    k_rope = partial_rope(k_rope, cos, sin)
    iq = partial_rope(iq, cos[:, None], sin[:, None]).astype(jnp.float32)
    ik = partial_rope(ik, cos, sin).astype(jnp.float32)
    q_abs = jnp.einsum('bshn,rhn->bshr', q_nope, w_uk)
    q_cat = jnp.concatenate([q_abs, q_rope], axis=-1).astype(jnp.float32) * DSA_HEAD_DIM ** -0.5
    kv_cat = jnp.concatenate([c_kv, k_rope], axis=-1).astype(jnp.float32)
    key_pos = jnp.arange(seq)
    starts = jnp.arange(0, seq, Q_BLOCK)

    def block(args):
        qb, iqb, iwb, start = args
        q_pos = start + jnp.arange(Q_BLOCK)
        causal = key_pos[None, :] <= q_pos[:, None]
        score = jnp.einsum('bth,bths->bts', iwb,
                           jax.nn.relu(jnp.einsum('bthd,bsd->bths', iqb, ik)))
        score = jnp.where(causal[None], score, -jnp.inf)
        _, sel = lax.top_k(score, topk)
        kv_sel = jax.vmap(lambda kv, ix: kv[ix])(kv_cat, sel)
        valid = sel <= q_pos[None, :, None]
        logits = jnp.einsum('bthf,btkf->bthk', qb, kv_sel)
        logits = jnp.where(valid[:, :, None, :], logits, -jnp.inf)
        p = jax.nn.softmax(logits, axis=-1)
        return jnp.einsum('bthk,btkr->bthr', p, kv_sel[..., :DSA_KV_RANK])

    o_lat = from_blocks(lax.map(block, (to_blocks(q_cat), to_blocks(iq),
                                        to_blocks(iw.astype(jnp.float32)), starts)))
    o = jnp.einsum('bshr,rhv->bshv', o_lat, w_uv)
    return o.reshape(bsz, seq, DSA_HEADS * DSA_V_DIM)


def stick_breaking_attention(q, k, v):
    bsz, seq = q.shape[:2]
    qs = q.astype(jnp.float32) * SB_HEAD_DIM ** -0.5
    k32 = k.astype(jnp.float32)
    v32 = v.astype(jnp.float32)
    key_pos = jnp.arange(seq)
    starts = jnp.arange(0, seq, Q_BLOCK)

    def block(args):
        qb, start = args
        q_pos = start + jnp.arange(Q_BLOCK)
        strict = key_pos[None, :] < q_pos[:, None]
        z = jnp.einsum('bthd,bshd->bhts', qb, k32)
        log_stay = jnp.where(strict, jax.nn.log_sigmoid(-z), 0.0)
        log_after = lax.cumsum(log_stay, axis=3, reverse=True) - log_stay
        a = jnp.where(strict, jnp.exp(jax.nn.log_sigmoid(z) + log_after), 0.0)
        return jnp.einsum('bhts,bshd->bthd', a, v32)

    o = from_blocks(lax.map(block, (to_blocks(qs), starts)))
    return o.reshape(bsz, seq, SB_HEADS * SB_HEAD_DIM)


def causal_depthwise_conv(u, w):
    return lax.conv_general_dilated(u, w[:, None, :].astype(u.dtype), window_strides=(1,),
                                    padding=[(ML_CONV - 1, 0)],
                                    dimension_numbers=('NWC', 'WIO', 'NWC'),
                                    feature_group_count=u.shape[-1])


def mlstm_chunkwise(q, k, v, i_pre, f_pre):
    bsz, seq = q.shape[:2]
    nc = seq // ML_CHUNK

    def chunked(a):
        a = jnp.moveaxis(a.astype(jnp.float32), 2, 1)
        a = a.reshape((bsz, ML_HEADS, nc, ML_CHUNK) + a.shape[3:])
        return jnp.moveaxis(a, 2, 0)

    qc = chunked(q)
    kc = chunked(k.astype(jnp.float32) * ML_HEAD_DIM ** -0.5)
    vc = chunked(v)
    ic = chunked(i_pre)
    lfc = chunked(jax.nn.log_sigmoid(f_pre.astype(jnp.float32)))
    tril = jnp.tril(jnp.ones((ML_CHUNK, ML_CHUNK), dtype=bool))

    def step(carry, inp):
        c_mem, n_mem, m_prev = carry
        qb, kb, vb, ib, lfb = inp
        b = jnp.cumsum(lfb, axis=-1)
        log_d = jnp.where(tril, b[..., :, None] - b[..., None, :] + ib[..., None, :], -jnp.inf)
        log_inter = b + m_prev[..., None]
        m_row = jnp.maximum(jnp.max(log_d, axis=-1), log_inter)
        w_intra = jnp.exp(log_d - m_row[..., None])
        w_inter = jnp.exp(log_inter - m_row)
        s = jnp.einsum('bhtd,bhsd->bhts', qb, kb) * w_intra
        num = (jnp.einsum('bhts,bhse->bhte', s, vb)
               + w_inter[..., None] * jnp.einsum('bhtd,bhde->bhte', qb, c_mem))
        den = jnp.sum(s, axis=-1) + w_inter * jnp.einsum('bhtd,bhd->bht', qb, n_mem)
        h = num / jnp.maximum(jnp.abs(den), jnp.exp(-m_row))[..., None]
        b_last = b[..., -1]
        log_w = b_last[..., None] - b + ib
        m_new = jnp.maximum(b_last + m_prev, jnp.max(log_w, axis=-1))
        w_upd = jnp.exp(log_w - m_new[..., None])
        decay = jnp.exp(b_last + m_prev - m_new)
        c_mem = decay[..., None, None] * c_mem + jnp.einsum('bhs,bhsd,bhse->bhde', w_upd, kb, vb)
        n_mem = decay[..., None] * n_mem + jnp.einsum('bhs,bhsd->bhd', w_upd, kb)
        return (c_mem, n_mem, m_new), h

    init = (jnp.zeros((bsz, ML_HEADS, ML_HEAD_DIM, ML_HEAD_DIM), jnp.float32),
            jnp.zeros((bsz, ML_HEADS, ML_HEAD_DIM), jnp.float32),
            jnp.zeros((bsz, ML_HEADS), jnp.float32))
    _, h = lax.scan(step, init, (qc, kc, vc, ic, lfc))
    h = jnp.moveaxis(h, 0, 2).reshape(bsz, ML_HEADS, seq, ML_HEAD_DIM)
    return jnp.moveaxis(h, 1, 2)


def head_layer_norm(h, w):
    h = h.astype(jnp.float32)
    mu = jnp.mean(h, axis=-1, keepdims=True)
    var = jnp.mean(jnp.square(h - mu), axis=-1, keepdims=True)
    y = (h - mu) * lax.rsqrt(var + NORM_EPS)
    return y.reshape(h.shape[0], h.shape[1], -1) * w.astype(jnp.float32)


def hybrid_layer(x, w_in, w_uk, w_uv, kv_norm_w, conv_w, i_bias, f_bias, ml_norm_w,
                 w_branch, w_out, norm_w, cos, sin):
    bsz, seq, _ = x.shape
    h = rms_norm(x, norm_w)
    p = h @ w_in
    splits = [int(c) for c in np.cumsum(IN_WIDTHS)[:-1]]
    (dsa_q, dsa_ckv, dsa_krope, idx_q, idx_k, idx_w, dsa_z,
     sb_q, sb_k, sb_v, sb_z,
     ml_qk, ml_v, ml_i, ml_f, ml_o, ml_z, merge) = jnp.split(p, splits, axis=-1)

    def heads(a, n):
        return a.reshape(bsz, seq, n, -1)

    y_a = dsa_attention(heads(dsa_q, DSA_HEADS), dsa_ckv, dsa_krope, heads(idx_q, IDX_HEADS),
                        idx_k, idx_w, w_uk, w_uv, kv_norm_w, cos, sin)
    y_b = stick_breaking_attention(heads(sb_q, SB_HEADS), heads(sb_k, SB_HEADS), heads(sb_v, SB_HEADS))
    ml_q, ml_k = jnp.split(jax.nn.silu(causal_depthwise_conv(ml_qk, conv_w)), 2, axis=-1)
    h_c = mlstm_chunkwise(heads(ml_q, ML_HEADS), heads(ml_k, ML_HEADS), heads(ml_v, ML_HEADS),
                          ml_i + i_bias, ml_f + f_bias)
    y_c = head_layer_norm(h_c, ml_norm_w) * jax.nn.sigmoid(ml_o.astype(jnp.float32))

    branches = jnp.stack([y_a * jax.nn.silu(dsa_z), y_b * jax.nn.silu(sb_z),
                          y_c * jax.nn.silu(ml_z)], axis=2)
    proj = jnp.einsum('bsgc,gcd->bsgd', branches, w_branch)
    gates = jax.nn.sigmoid(merge.reshape(bsz, seq, N_BRANCHES, D_MODEL).astype(jnp.float32))
    mixed = jnp.sum(gates * proj, axis=2)
    return x + (mixed @ w_out).astype(x.dtype)


def setup_inputs(seed: int = 0) -> dict:
    key = jax.random.key(seed)
    ks = jax.random.split(key, 13)

    def nrm(k, shape, fan_in):
        return jax.random.normal(k, shape, jnp.float32) * fan_in ** -0.5

    x = jax.random.normal(ks[0], (BATCH, SEQ, D_MODEL), jnp.float32)
    w_in = nrm(ks[1], (DEPTH, D_MODEL, W_IN_COLS), D_MODEL)
    w_dsa_uk = nrm(ks[2], (DEPTH, DSA_KV_RANK, DSA_HEADS, DSA_NOPE_DIM), DSA_KV_RANK)
    w_dsa_uv = nrm(ks[3], (DEPTH, DSA_KV_RANK, DSA_HEADS, DSA_V_DIM), DSA_KV_RANK)
    dsa_kv_norm_w = 1.0 + 0.02 * jax.random.normal(ks[4], (DEPTH, DSA_KV_RANK), jnp.float32)
    ml_conv_w = nrm(ks[5], (DEPTH, ML_CONV, 2 * ML_WIDTH), ML_CONV)
    ml_i_bias = 0.1 * jax.random.normal(ks[6], (DEPTH, ML_HEADS), jnp.float32)
    ml_f_bias = (jnp.linspace(3.0, 6.0, ML_HEADS, dtype=jnp.float32)[None, :]
                 + 0.1 * jax.random.normal(ks[7], (DEPTH, ML_HEADS), jnp.float32))
    ml_norm_w = 1.0 + 0.02 * jax.random.normal(ks[8], (DEPTH, ML_WIDTH), jnp.float32)
    w_branch = nrm(ks[9], (DEPTH, N_BRANCHES, BRANCH_WIDTH, D_MODEL), BRANCH_WIDTH)
    w_out = nrm(ks[10], (DEPTH, D_MODEL, D_MODEL), D_MODEL)
    norm_w = 1.0 + 0.02 * jax.random.normal(ks[11], (DEPTH, D_MODEL), jnp.float32)
    final_norm_w = 1.0 + 0.02 * jax.random.normal(ks[12], (D_MODEL,), jnp.float32)
    return {'x': x, 'w_in': w_in, 'w_dsa_uk': w_dsa_uk, 'w_dsa_uv': w_dsa_uv,
            'dsa_kv_norm_w': dsa_kv_norm_w, 'ml_conv_w': ml_conv_w, 'ml_i_bias': ml_i_bias,
            'ml_f_bias': ml_f_bias, 'ml_norm_w': ml_norm_w, 'w_branch': w_branch,
            'w_out': w_out, 'norm_w': norm_w, 'final_norm_w': final_norm_w}


def reference(x, w_in, w_dsa_uk, w_dsa_uv, dsa_kv_norm_w, ml_conv_w, ml_i_bias, ml_f_bias,
              ml_norm_w, w_branch, w_out, norm_w, final_norm_w):
    cos, sin = rope_tables(x.shape[1])
    for l in range(DEPTH):
        x = hybrid_layer(x, w_in[l], w_dsa_uk[l], w_dsa_uv[l], dsa_kv_norm_w[l], ml_conv_w[l],
                         ml_i_bias[l], ml_f_bias[l], ml_norm_w[l], w_branch[l], w_out[l],
                         norm_w[l], cos, sin)
    return rms_norm(x, final_norm_w)
```

```python
import numpy as np
import concourse.bass as bass
import concourse.mybir as mybir
from concourse.bass_utils import run_bass_kernel_spmd

F32 = mybir.dt.float32
BF16 = mybir.dt.bfloat16
AF = mybir.ActivationFunctionType
ALU = mybir.AluOpType
AX = mybir.AxisListType

NDMA = 24
EPS = 1e-6
NEG = -30000.0

COLS = dict(dsa_q=(0, 512), ckv=(512, 128), krope=(640, 16), idx_q=(656, 256), idx_k=(912, 64),
            idx_w=(976, 4), dsa_z=(980, 512), sb_q=(1492, 512), sb_k=(2004, 512), sb_v=(2516, 512),
            sb_z=(3028, 512), ml_q=(3540, 512), ml_k=(4052, 512), ml_v=(4564, 512), ml_i=(5076, 4),
            ml_f=(5080, 4), ml_o=(5084, 512), ml_z=(5596, 512), merge=(6108, 3072))


class T:
    def __init__(self, ap, key):
        self.ap, self.key = ap, key

    def __getitem__(self, idx):
        return T(self.ap[idx], self.key)

    def re(self, pat, **kw):
        return T(self.ap.rearrange(pat, **kw), self.key)

    def bc(self, shape):
        return T(self.ap.to_broadcast(shape), self.key)


class Prog:
    ENG = ("pe", "act", "dve", "pool", "sp")

    def __init__(self, nc):
        self.nc = nc
        self.ops = {e: [] for e in self.ENG}
        self.cnt = {e: 0 for e in self.ENG}
        self.seen = {e: {} for e in self.ENG}
        self.last_w = {}
        self.readers = {}
        self.ndma = 0
        self.dma_last = {}

    def _need(self, reads, writes):
        need = {}

        def add(s, v):
            if need.get(s, 0) < v:
                need[s] = v
        for k in reads:
            ev = self.last_w.get(k)
            if ev:
                add(*ev)
        for k in writes:
            ev = self.last_w.get(k)
            if ev:
                add(*ev)
            for s, v in self.readers.get(k, {}).items():
                add(s, v)
        return need

    def _commit(self, ev, reads, writes):
        for k in reads:
            d = self.readers.setdefault(k, {})
            if d.get(ev[0], 0) < ev[1]:
                d[ev[0]] = ev[1]
        for k in writes:
            self.last_w[k] = ev
            self.readers[k] = {}

    def _waits(self, eng, need, skip_pe=False):
        waits = []
        for s, v in need.items():
            if skip_pe and s == "pe":
                continue
            if self.seen[eng].get(s, 0) < v:
                self.seen[eng][s] = v
                waits.append((s, v))
        return waits

    def op(self, eng, fn, reads=(), writes=(), pe_chain=False):
        waits = self._waits(eng, self._need(reads, writes), pe_chain)
        self.cnt[eng] += 1
        ev = (eng, self.cnt[eng])
        self.ops[eng].append((waits, fn, ev))
        self._commit(ev, reads, writes)
        return ev

    def dma(self, eng, fn, reads=(), writes=()):
        need = self._need(reads, writes)
        k = self.ndma % NDMA
        self.ndma += 1
        sk = "dma%d" % k
        prev = self.dma_last.get(k, 0)
        if prev and need.get(sk, 0) < prev:
            need[sk] = prev
        waits = self._waits(eng, need)
        ev = (sk, prev + 16)
        self.dma_last[k] = prev + 16
        self.ops[eng].append((waits, fn, ev))
        self._commit(ev, reads, writes)
        return ev

    def barrier(self):
        evs = [(e, self.cnt[e]) for e in self.ENG if self.cnt[e]]
        evs += [("dma%d" % k, v) for k, v in self.dma_last.items()]
        for e in self.ENG:
            w = self._waits(e, dict(evs))
            if w:
                self.ops[e].append((w, None, None))

    def I(self, eng, meth, outs, ins, pe_chain=False, **kw):
        args = dict(kw)
        reads, writes = [], []
        for k, v in outs.items():
            args[k] = v.ap
            writes.append(v.key)
        for k, v in ins.items():
            if isinstance(v, T):
                args[k] = v.ap
                reads.append(v.key)
            else:
                args[k] = v
        return self.op(eng, lambda e: getattr(e, meth)(**args), reads, writes, pe_chain)

    def mm(self, out, lhsT, rhs, start=True, stop=True):
        o, a, b = out.ap, lhsT.ap, rhs.ap
        return self.op("pe", lambda e: e.matmul(o, lhsT=a, rhs=b, start=start, stop=stop),
                       [lhsT.key, rhs.key], [out.key], pe_chain=True)

    def tr(self, out, in_, ident):
        o, a, b = out.ap, in_.ap, ident.ap
        return self.op("pe", lambda e: e.transpose(out=o, in_=a, identity=b),
                       [in_.key, ident.key], [out.key], pe_chain=True)

    def D(self, eng, out, in_):
        reads, writes = [], []
        o = out.ap if isinstance(out, T) else out
        i = in_.ap if isinstance(in_, T) else in_
        if isinstance(out, T):
            writes.append(out.key)
        if isinstance(in_, T):
            reads.append(in_.key)
        return self.dma(eng, lambda e: e.dma_start(out=o, in_=i), reads, writes)

    def Dk(self, eng, out, in_, reads=(), writes=()):
        r, w = list(reads), list(writes)
        o = out.ap if isinstance(out, T) else out
        i = in_.ap if isinstance(in_, T) else in_
        if isinstance(out, T):
            w.append(out.key)
        if isinstance(in_, T):
            r.append(in_.key)
        return self.dma(eng, lambda e: e.dma_start(out=o, in_=i), r, w)

    def emit(self, stack):
        nc = self.nc
        sems = {}
        for e in self.ENG:
            sems[e] = stack.enter_context(nc.semaphore("s_" + e))
        for k in range(NDMA):
            sems["dma%d" % k] = stack.enter_context(nc.semaphore("s_dma%d" % k))
        block = stack.enter_context(nc.Block())

        waited = {e: set() for e in self.ENG}
        for e in self.ENG:
            for waits, fn, ev in self.ops[e]:
                for s, v in waits:
                    if s in waited:
                        waited[s].add(v)
        rank = {e: {v: i + 1 for i, v in enumerate(sorted(waited[e]))} for e in self.ENG}

        def run(name):
            def body(eng):
                for waits, fn, ev in self.ops[name]:
                    for s, v in waits:
                        eng.wait_ge(sems[s], rank[s][v] if s in rank else v)
                    if fn is None:
                        continue
                    ins = fn(eng)
                    s, v = ev
                    if s.startswith("dma"):
                        ins.then_inc(sems[s], 16)
                    elif v in rank[s]:
                        ins.then_inc(sems[s], 1)
            return body
        block.tensor(run("pe"))
        block.scalar(run("act"))
        block.vector(run("dve"))
        block.gpsimd(run("pool"))
        block.sync(run("sp"))


class Alloc:
    def __init__(self, nc, base, tag):
        self.nc, self.off, self.tag, self.n = nc, base, tag, 0

    def __call__(self, shape, dt, key=None):
        per = int(np.prod(shape[1:])) * (4 if dt == F32 else 2)
        per = (per + 31) // 32 * 32
        name = "%s_%d" % (self.tag, self.n)
        self.n += 1
        t = self.nc.alloc_sbuf_tensor_at(name, list(shape), dt, offset=self.off)
        self.off += per
        assert self.off <= 229000, (self.tag, self.off)
        return T(t.ap(), key or name)


CBASE = 16640
PH0 = CBASE + 20480


class Layer:
    def __init__(self, last, debug=False, phases=("A", "B", "sb", "ml", "dsa", "tail"), nblk=16):
        self.last, self.debug, self.phases, self.nblk = last, debug, phases, nblk
        nc = self.nc = bass.Bass("TRN2", target_bir_lowering=False)
        self.P = Prog(nc)
        inp = lambda n, s: nc.dram_tensor(n, list(s), F32, kind="ExternalInput").ap()
        self.x_all = inp("x_all", [4096, 1024])
        self.x_own = inp("x_own", [2048, 1024])
        self.w_in = inp("w_in", [1024, 9180])
        self.wukT = inp("wukT", [128, 512])
        self.w_uv = inp("w_uv", [128, 512])
        self.w_branch = inp("w_branch", [3, 512, 1024])
        self.w_out = inp("w_out", [1024, 1024])
        self.nwb = inp("nwb", [128, 1024])
        self.fnwb = inp("fnwb", [128, 1024])
        self.small = inp("small", [128, 64])
        self.cf = inp("cf", [128, 512])
        self.cb = inp("cb", [128, 1024])
        self.msk = inp("msk", [128, 3072])
        self.mq = inp("mq", [128, 256])
        self.sel = inp("sel", [4, 512])
        self.ropeA = inp("ropeA", [2, 128, 4096])
        self.ropeO = inp("ropeO", [2, 128, 16, 512])
        dbg = debug if isinstance(debug, (tuple, list, set)) else ()
        scr = lambda n, s, dt: nc.dram_tensor(n, list(s), dt, kind=("ExternalOutput" if (debug is True or n in dbg) else "Internal")).ap()
        self.SBK = scr("SBK", [512, 4096], BF16)
        self.SBV = scr("SBV", [4096, 512], BF16)
        self.UK = scr("UK", [512, 4096], F32)
        self.UQ = scr("UQ", [512, 4096], F32)
        self.MLV = scr("MLV", [4096, 512], BF16)
        self.CKVT = scr("CKVT", [128, 4096], F32)
        self.KRT = scr("KRT", [16, 4096], F32)
        self.IKT = scr("IKT", [64, 4096], F32)
        self.GI = scr("GI", [4, 4096], F32)
        self.GF = scr("GF", [4, 4096], F32)
        self.DQT = scr("DQT", [512, 2048], F32)
        self.IQT = scr("IQT", [256, 2048], F32)
        self.DZT = scr("DZT", [512, 2048], BF16)
        self.SBQ = scr("SBQ", [512, 2048], BF16)
        self.SBZ = scr("SBZ", [512, 2048], BF16)
        self.MLO = scr("MLO", [512, 2048], BF16)
        self.MLZ = scr("MLZ", [512, 2048], BF16)
        self.MRG = scr("MRG", [3072, 2048], BF16)
        self.IWS = scr("IWS", [2048, 4], F32)
        self.BR = scr("BR", [3, 512, 2048], BF16)
        self.out = nc.dram_tensor("out", [2048, 1024], F32, kind="ExternalOutput").ap()
        self.B = [T(nc.alloc_psum_tensor("bank%d" % i, [128, 512], F32).ap(), "B%d" % i) for i in range(8)]
        self.consts()

    def consts(self):
        P, nc = self.P, self.nc
        al = Alloc(nc, CBASE, "c")
        self.CF = al([128, 512], F32)
        self.IDF, self.PROT = self.CF[:, 0:128], self.CF[:, 128:256]
        self.ONESF, self.ONES128 = self.CF[:, 256:384], self.CF[:, 384:512]
        self.CB = al([128, 1024], BF16)
        self.IDB, self.ONESB = self.CB[:, 0:128], self.CB[:, 128:256]
        self.NEGONES, self.NEGTRI, self.I4 = self.CB[:, 256:384], self.CB[:, 384:512], self.CB[:, 512:1024]
        self.MSK = al([128, 3072], BF16)
        self.MQ = al([128, 256], F32)
        self.SEL = al([4, 512], F32)
        self.NWB = al([128, 1024], F32)
        self.SM = al([128, 64], F32)
        assert al.off <= PH0, al.off
        P.D("sp", self.CF, self.cf)
        P.D("pool", self.CB, self.cb)
        P.D("pool", self.MSK, self.msk)
        P.D("sp", self.MQ, self.mq)
        P.D("sp", self.SEL, self.sel)
        P.D("sp", self.NWB, self.nwb)
        P.D("sp", self.SM, self.small)
        P.I("dve", "tensor_scalar", dict(out=self.SM[0:4, 35:36]), dict(in0=self.SM[0:4, 34:35]),
            scalar1=-1.0, scalar2=None, op0=ALU.mult)
        P.I("dve", "tensor_scalar", dict(out=self.SM[0:4, 33:34]), dict(in0=self.SM[0:4, 33:34]),
            scalar1=float(-0.5 * np.log(128.0)), scalar2=None, op0=ALU.add)
        P.I("dve", "tensor_scalar", dict(out=self.SM[:, 42:44]), dict(in0=self.SM[:, 40:42]),
            scalar1=-1.0, scalar2=None, op0=ALU.mult)

    def load_w(self, W, segs):
        P = self.P
        wv = self.w_in.rearrange("(kc p) n -> p kc n", p=128)
        col = 0
        offs = []
        for c0, n in segs:
            offs.append(col)
            for a in range(0, n, 512):
                nn = min(512, n - a)
                P.D("pool", W[:, :, col + a:col + a + nn], wv[:, :, c0 + a:c0 + a + nn])
            col += n
        return offs

    def make_hT(self, al_tmp, xsrc, nblk, hT):
        P = self.P
        XT, XS, JUNK, SS = al_tmp
        for bi in range(nblk):
            xt = XT[bi % 2]
            P.D("sp", xt, xsrc[bi * 128:(bi + 1) * 128, :])
            P.I("act", "activation", dict(out=JUNK, accum_out=SS[:, 0:1]), dict(in_=xt), func=AF.Square)
            P.I("act", "activation", dict(out=SS[:, 1:2]), dict(in_=SS[:, 0:1]), func=AF.Sqrt, scale=1.0 / 1024, bias=EPS)
            P.I("dve", "reciprocal", dict(out=SS[:, 2:3]), dict(in_=SS[:, 1:2]))
            P.I("dve", "scalar_tensor_tensor", dict(out=XS), dict(in0=xt, scalar=SS[:, 2:3], in1=self.NWB),
                op0=ALU.mult, op1=ALU.mult)
            for q in range(2):
                pb = self.B[6 + q]
                for k4 in range(4):
                    k = q * 4 + k4
                    P.tr(pb[:, k4 * 128:(k4 + 1) * 128], XS[:, k * 128:(k + 1) * 128], self.IDF)
                dst = hT[:, q * 4:q * 4 + 4, bi * 128:(bi + 1) * 128]
                src = pb.re("p (a b) -> p a b", a=4)
                if q == 0:
                    P.I("act", "activation", dict(out=dst), dict(in_=src), func=AF.Copy)
                else:
                    P.I("dve", "tensor_copy", dict(out=dst), dict(in_=src))

    def proj_pass(self, xsrc, ntok, fm_segs, tm_segs, tag):
        P, nc = self.P, self.nc
        P.barrier()
        al = Alloc(nc, PH0, tag)
        ncols = sum(s[2] for s in fm_segs) + sum(s[2] for s in tm_segs)
        W = al([128, 8, ncols], BF16)
        offs = self.load_w(W, [(s[1], s[2]) for s in fm_segs] + [(s[1], s[2]) for s in tm_segs])
        HT = [al([128, 8, 512], BF16) for _ in range(2)]
        XT = [al([128, 1024], F32) for _ in range(2)]
        XS = al([128, 1024], F32)
        JUNK = al([128, 1024], BF16)
        SS = al([128, 8], F32)
        STF = [al([128, 512], F32) for _ in range(3)]
        STB = [al([128, 512], BF16) for _ in range(3)]
        ngrp = ntok // 512
        ev = 0
        for g in range(ngrp):
            hT = HT[g % 2]
            self.make_hT((XT, XS, JUNK, SS), xsrc[g * 512:(g + 1) * 512, :], 4, hT)
            for si, (name, c0, n, dst, dt, func, scale) in enumerate(fm_segs):
                for a in range(0, n, 128):
                    nn = min(128, n - a)
                    pb = self.B[ev % 4]
                    for k in range(8):
                        P.mm(pb[0:nn, :], W[:, k, offs[si] + a:offs[si] + a + nn], hT[:, k, :], start=(k == 0), stop=(k == 7))
                    st = (STF if dt == F32 else STB)[ev % 3]
                    if func is None and ev % 2 == 0 and scale == 1.0:
                        P.I("dve", "tensor_copy", dict(out=st[0:nn, :]), dict(in_=pb[0:nn, :]))
                    else:
                        P.I("act", "activation", dict(out=st[0:nn, :]), dict(in_=pb[0:nn, :]),
                            func=(func or AF.Copy), scale=scale)
                    P.Dk("sp", dst[a:a + nn, g * 512:(g + 1) * 512], st[0:nn, :], writes=[name])
                    ev += 1
            nf = len(fm_segs)
            for si, (name, c0, n, dst, dt) in enumerate(tm_segs):
                for bi in range(4):
                    for a in range(0, n, 512):
                        nn = min(512, n - a)
                        pb = self.B[ev % 4]
                        o = offs[nf + si] + a
                        for k in range(8):
                            P.mm(pb[:, 0:nn], hT[:, k, bi * 128:(bi + 1) * 128], W[:, k, o:o + nn], start=(k == 0), stop=(k == 7))
                        st = (STF if dt == F32 else STB)[ev % 3]
                        if ev % 2 == 0:
                            P.I("dve", "tensor_copy", dict(out=st[:, 0:nn]), dict(in_=pb[:, 0:nn]))
                        else:
                            P.I("act", "activation", dict(out=st[:, 0:nn]), dict(in_=pb[:, 0:nn]), func=AF.Copy)
                        r0 = g * 512 + bi * 128
                        P.Dk("sp", dst[r0:r0 + 128, a:a + nn], st[:, 0:nn], writes=[name])
                        ev += 1

    def pass_A(self):
        C = COLS
        fm = [("SBK", *C["sb_k"], self.SBK, BF16, None, 1.0), ("UK", *C["ml_k"], self.UK, F32, None, 1.0),
              ("UQ", *C["ml_q"], self.UQ, F32, None, 1.0), ("CKVT", *C["ckv"], self.CKVT, F32, None, 1.0),
              ("KRT", *C["krope"], self.KRT, F32, None, 1.0), ("IKT", *C["idx_k"], self.IKT, F32, None, 1.0),
              ("GI", *C["ml_i"], self.GI, F32, None, 1.0), ("GF", *C["ml_f"], self.GF, F32, None, 1.0)]
        tm = [("SBV", *C["sb_v"], self.SBV, BF16), ("MLV", *C["ml_v"], self.MLV, BF16)]
        self.proj_pass(self.x_all, 4096, fm, tm, "pa")

    def pass_B(self):
        C = COLS
        fm = [("DQT", *C["dsa_q"], self.DQT, F32, None, 1.0), ("IQT", *C["idx_q"], self.IQT, F32, None, 1.0),
              ("DZT", *C["dsa_z"], self.DZT, BF16, AF.Silu, 1.0), ("SBQ", *C["sb_q"], self.SBQ, BF16, None, 0.125),
              ("SBZ", *C["sb_z"], self.SBZ, BF16, AF.Silu, 1.0), ("MLO", *C["ml_o"], self.MLO, BF16, AF.Sigmoid, 1.0),
              ("MLZ", *C["ml_z"], self.MLZ, BF16, AF.Silu, 1.0), ("MRG", *C["merge"], self.MRG, BF16, AF.Sigmoid, 1.0)]
        tm = [("IWS", *C["idx_w"], self.IWS, F32)]
        self.proj_pass(self.x_own, 2048, fm, tm, "pb")

    def phase_sb(self):
        P, nc, B = self.P, self.nc, self.B
        P.barrier()
        al = Alloc(nc, PH0, "sb")
        KA = al([128, 4, 4096], BF16)
        VA = al([128, 32, 512], BF16)
        QA = al([128, 4, 2048], BF16)
        YA = al([128, 4, 2048], BF16)
        QM = al([128, 8, 2048], BF16)
        E = [al([128, 512], F32) for _ in range(2)]
        SP = [al([128, 512], BF16) for _ in range(2)]
        AT = [al([128, 512], BF16) for _ in range(2)]
        SACC = al([128, 512], F32)
        SACCB = al([128, 512], BF16)
        for pr in range(4):
            P.Dk("sp", KA[:, pr, :], self.SBK[pr * 128:(pr + 1) * 128, :], reads=["SBK"])
            P.Dk("sp", QA[:, pr, :], self.SBQ[pr * 128:(pr + 1) * 128, :], reads=["SBQ"])
        vv = self.SBV.rearrange("(j p) n -> p j n", p=128)
        for q in range(4):
            P.Dk("sp", VA[:, q * 8:(q + 1) * 8, :], vv[:, q * 8:(q + 1) * 8, :], reads=["SBV"])
        MUL = self.MSK[:, 0:1024].re("p (a b) -> p a b", a=2)
        ADD = self.MSK[:, 1024:2048].re("p (a b) -> p a b", a=2)
        qm4 = QM.re("p (a e) t -> p a e t", e=2)
        for e in range(2):
            P.I("dve", "tensor_scalar", dict(out=qm4[:, :, e, :]), dict(in0=QA, scalar1=self.SM[:, 44 + e:45 + e]),
                scalar2=None, op0=ALU.mult)
        tix = 0
        gix = 0
        import os
        SBX = int(os.environ.get("SBX", "9"))
        for m in range(self.nblk if SBX > 1 else 0):
            for hg in range(2):
                po = B[4 + gix % 2]
                gix += 1
                nj = 2 * m + 2
                for jj in range(nj):
                    j = nj - 1 - jj
                    s = tix % 2
                    tix += 1
                    pz, pa = B[s], B[2 + s]
                    heads = []
                    for hh in range(4):
                        h = 4 * hg + hh
                        pr, hb = h // 2, (h % 2) * 64
                        heads.append((hh, h, pr, hb))
                    for hh, h, pr, hb in heads:
                        P.mm(pz[:, hh * 128:(hh + 1) * 128], KA[:, pr, j * 128:(j + 1) * 128],
                             QM[:, h, m * 128:(m + 1) * 128])
                    P.I("act", "activation", dict(out=E[s]), dict(in_=pz), func=AF.Exp)
                    P.I("act", "activation", dict(out=SP[s]), dict(in_=E[s]), func=AF.Ln, bias=1.0)
                    mi = j - 2 * m
                    if SBX < 3:
                        continue
                    if mi >= 0:
                        P.I("pool", "tensor_tensor", dict(out=SP[s]), dict(in0=SP[s], in1=MUL[:, mi, :]), op=ALU.mult)
                    for hh, h, pr, hb in heads:
                        P.mm(pa[:, hh * 128:(hh + 1) * 128], KA[:, pr, j * 128:(j + 1) * 128],
                             QM[:, h, m * 128:(m + 1) * 128], start=(hh == 0), stop=False)
                    extra = [(self.NEGTRI, SP[s])]
                    if jj > 0:
                        extra.append((self.NEGONES, SACCB))
                    if mi >= 0:
                        extra.append((self.IDB, ADD[:, mi, :]))
                    for ei, (l, r) in enumerate(extra):
                        P.mm(pa, l, r, start=False, stop=(ei == len(extra) - 1))
                    P.I("act", "activation", dict(out=AT[s]), dict(in_=pa), func=AF.Exp)
                    if SBX < 4:
                        continue
                    for hh, h, pr, hb in heads:
                        P.mm(po[hb:hb + 64, (pr - 2 * hg) * 128:(pr - 2 * hg + 1) * 128],
                             VA[:, j, h * 64:(h + 1) * 64], AT[s][:, hh * 128:(hh + 1) * 128],
                             start=(jj == 0 and hh < 2), stop=(jj == nj - 1))
                    if jj < nj - 1:
                        if jj == 0:
                            P.I("pool", "tensor_copy", dict(out=SACC), dict(in_=SP[s]))
                        else:
                            P.I("pool", "tensor_tensor", dict(out=SACC), dict(in0=SACC, in1=SP[s]), op=ALU.add)
                        P.I("pool", "tensor_copy", dict(out=SACCB), dict(in_=SACC))
                if SBX < 4:
                    continue
                P.I("dve", "tensor_copy", dict(out=YA[:, 2 * hg:2 * hg + 2, m * 128:(m + 1) * 128]),
                    dict(in_=po[:, 0:256].re("p (a b) -> p a b", a=2)))
        for pr in range(4):
            P.Dk("sp", QA[:, pr, :], self.SBZ[pr * 128:(pr + 1) * 128, :], reads=["SBZ"])
        P.I("dve", "tensor_tensor", dict(out=YA), dict(in0=YA, in1=QA), op=ALU.mult)
        for pr in range(4):
            P.Dk("sp", self.BR[1, pr * 128:(pr + 1) * 128, :], YA[:, pr, :], writes=["BR"])

    def phase_ml(self):
        P, nc, B = self.P, self.nc, self.B
        P.barrier()
        al = Alloc(nc, PH0, "ml")
        KA = al([128, 4, 4096], BF16)
        VA = al([128, 32, 512], BF16)
        QA = al([128, 4, 2048], BF16)
        YA = al([128, 4, 2048], BF16)
        GC = al([128, 4, 2048], BF16)
        G1 = al([4, 4096], F32)
        RQ = al([4, 2048], F32)
        ACC = al([128, 4096], F32)
        fb_off = al.off
        FB = al([128, 4100], F32)
        G2 = G1
        AR = G1
        CN = T(ACC.ap[0:4, :], ACC.key)
        SM = self.SM
        vv = self.MLV.rearrange("(j p) n -> p j n", p=128)
        for q in range(4):
            P.Dk("sp", VA[:, q * 8:(q + 1) * 8, :], vv[:, q * 8:(q + 1) * 8, :], reads=["MLV"])
        P.Dk("sp", G1, self.GF, reads=["GF"])
        P.I("act", "activation", dict(out=G1), dict(in_=G1, bias=SM[0:4, 35:36]), func=AF.Exp, scale=-1.0)
        P.I("act", "activation", dict(out=G2), dict(in_=G1), func=AF.Ln, bias=1.0)
        P.I("dve", "tensor_tensor_scan", dict(out=CN), dict(data0=G2, data1=G2), initial=0.0, op0=ALU.add, op1=ALU.bypass)
        P.Dk("sp", G1, self.GI, reads=["GI"])
        P.I("dve", "scalar_tensor_tensor", dict(out=AR), dict(in0=G1, scalar=SM[0:4, 33:34], in1=CN), op0=ALU.add, op1=ALU.add)
        cn4 = CN.re("p (m two t) -> p m two t", two=2, t=128)
        rq3 = RQ.re("p (m t) -> p m t", t=128)
        P.I("dve", "tensor_scalar", dict(out=rq3), dict(in0=cn4[:, :, 0, :], scalar1=SM[0:4, 42:43]), scalar2=None, op0=ALU.mult)
        P.I("dve", "scalar_tensor_tensor", dict(out=rq3), dict(in0=cn4[:, :, 1, :], scalar=SM[0:4, 43:44], in1=rq3),
            op0=ALU.mult, op1=ALU.add)
        P.I("pool", "memset", dict(ap=FB[:, 0:4]), {}, constant=0.0)
        for tix in range(8):
            isq = tix < 4
            src = (self.UQ if isq else self.UK)[(tix % 4) * 128:(tix % 4 + 1) * 128, :]
            P.Dk("sp", FB[:, 3:4099], src, reads=["UQ" if isq else "UK"])
            cw = lambda tap: SM[:, 1 + tix * 4 + tap:2 + tix * 4 + tap]
            P.I("dve", "tensor_scalar", dict(out=ACC), dict(in0=FB[:, 3:4099], scalar1=cw(3)), scalar2=None, op0=ALU.mult)
            for tap in range(3):
                P.I("dve", "scalar_tensor_tensor", dict(out=ACC), dict(in0=FB[:, tap:tap + 4096], scalar=cw(tap), in1=ACC),
                    op0=ALU.mult, op1=ALU.add)
            if isq:
                P.I("act", "activation", dict(out=ACC), dict(in_=ACC), func=AF.Silu)
                a4 = ACC.re("p (m two t) -> p m two t", two=2, t=128)
                q3 = QA[:, tix, :].re("p (m t) -> p m t", t=128)
                P.I("dve", "tensor_scalar", dict(out=q3), dict(in0=a4[:, :, 0, :], scalar1=SM[:, 40:41]), scalar2=None, op0=ALU.mult)
                P.I("dve", "scalar_tensor_tensor", dict(out=q3), dict(in0=a4[:, :, 1, :], scalar=SM[:, 41:42], in1=q3),
                    op0=ALU.mult, op1=ALU.add)
            else:
                P.I("act", "activation", dict(out=KA[:, tix - 4, :]), dict(in_=ACC), func=AF.Silu)
        for pr in range(4):
            P.Dk("sp", GC[:, pr, :], self.MLO[pr * 128:(pr + 1) * 128, :], reads=["MLO"])
            P.Dk("sp", YA[:, pr, :], self.MLZ[pr * 128:(pr + 1) * 128, :], reads=["MLZ"])
        P.I("pool", "tensor_tensor", dict(out=GC), dict(in0=GC, in1=YA), op=ALU.mult)
        MI = self.MSK[:, 2048:3072].re("p (a b) -> p a b", a=2)
        P.barrier()
        al2 = Alloc(nc, fb_off, "ml2")
        EW = [al2([128, 512], BF16) for _ in range(2)]
        PT = [al2([128, 512], BF16) for _ in range(2)]
        DN = al2([128, 512], F32)
        HH = al2([128, 512], F32)
        CEN = al2([128, 512], F32)
        SQ = al2([128, 512], F32)
        RSTD = al2([128, 512], F32)
        tix = 0
        for m in range(self.nblk):
            nj = 2 * m + 2
            pnum, pden = B[4], B[5]
            for j in range(nj):
                s = tix % 2
                tix += 1
                ps, pe = B[s], B[2 + s]
                for h in range(4):
                    P.mm(ps[:, h * 128:(h + 1) * 128], KA[:, h, j * 128:(j + 1) * 128], QA[:, h, m * 128:(m + 1) * 128])
                for h in range(4):
                    P.mm(pe[:, h * 128:(h + 1) * 128], self.SEL[0:4, h * 128:(h + 1) * 128], RQ[0:4, m * 128:(m + 1) * 128],
                         start=True, stop=False)
                    P.mm(pe[:, h * 128:(h + 1) * 128], AR[0:4, j * 128:(j + 1) * 128], self.SEL[0:4, h * 128:(h + 1) * 128],
                         start=False, stop=True)
                P.I("act", "activation", dict(out=EW[s]), dict(in_=pe), func=AF.Exp)
                P.I("dve", "tensor_tensor", dict(out=PT[s]), dict(in0=ps, in1=EW[s]), op=ALU.mult)
                mi = j - 2 * m
                if mi >= 0:
                    P.I("pool", "tensor_tensor", dict(out=PT[s]), dict(in0=PT[s], in1=MI[:, mi, :]), op=ALU.mult)
                for h in range(4):
                    P.mm(pnum[:, h * 128:(h + 1) * 128], VA[:, j, h * 128:(h + 1) * 128], PT[s][:, h * 128:(h + 1) * 128],
                         start=(j == 0 and h == 0), stop=(j == nj - 1))
                P.mm(pden, self.ONESB, PT[s], start=(j == 0), stop=(j == nj - 1))
            P.I("act", "activation", dict(out=DN), dict(in_=pden), func=AF.Abs)
            P.I("dve", "tensor_scalar", dict(out=DN), dict(in0=DN), scalar1=1.0, scalar2=None, op0=ALU.max)
            P.I("dve", "reciprocal", dict(out=DN), dict(in_=DN))
            P.I("dve", "tensor_tensor", dict(out=HH), dict(in0=pnum, in1=DN), op=ALU.mult)
            P.mm(B[6], self.ONES128, HH)
            P.I("dve", "tensor_tensor", dict(out=CEN), dict(in0=HH, in1=B[6]), op=ALU.subtract)
            P.I("act", "activation", dict(out=SQ), dict(in_=CEN), func=AF.Square)
            P.mm(B[7], self.ONES128, SQ)
            P.I("act", "activation", dict(out=RSTD), dict(in_=B[7]), func=AF.Sqrt, bias=EPS)
            P.I("dve", "reciprocal", dict(out=RSTD), dict(in_=RSTD))
            P.I("dve", "tensor_tensor", dict(out=CEN), dict(in0=CEN, in1=RSTD), op=ALU.mult)
            for h in range(4):
                P.I("dve", "scalar_tensor_tensor", dict(out=YA[:, h, m * 128:(m + 1) * 128]),
                    dict(in0=CEN[:, h * 128:(h + 1) * 128], scalar=SM[:, 36 + h:37 + h], in1=GC[:, h, m * 128:(m + 1) * 128]),
                    op0=ALU.mult, op1=ALU.mult)
        for pr in range(4):
            P.Dk("sp", self.BR[2, pr * 128:(pr + 1) * 128, :], YA[:, pr, :], writes=["BR"])

    def phase_dsa(self, nbis=22):
        P, nc, B = self.P, self.nc, self.B
        P.barrier()
        al = Alloc(nc, PH0, "ds")
        KVT = al([128, 4096], BF16)
        KR2 = al([128, 4096], BF16)
        IK2 = al([128, 4096], BF16)
        CKV = al([128, 32, 128], BF16)
        MA = al([128, 4096], BF16)
        FB = al([128, 4096], F32)
        F2 = al([128, 4096], F32)
        WUKT = al([128, 4, 128], BF16)
        WUV = al([128, 512], BF16)
        TA = [al([128, 512], F32) for _ in range(2)]
        T1 = al([128, 512], F32)
        T2 = al([128, 512], F32)
        DQR = al([128, 4, 128], F32)
        DQ = al([128, 4, 128], BF16)
        DQM = al([128, 8, 128], BF16)
        IQM = al([128, 4, 128], BF16)
        QABS = al([128, 8, 128], BF16)
        IQR = al([128, 2, 128], F32)
        IQ = al([128, 2, 128], BF16)
        IW = al([128, 4], F32)
        BS = al([128, 64], F32)
        MX = al([128, 8], F32)
        RD = al([128, 8, 128], BF16)
        PTt = [al([128, 512], BF16) for _ in range(2)]
        RS = al([128, 512], F32)
        OLN = al([128, 4, 128], BF16)
        YB = al([128, 4, 128], BF16)
        GZ = al([128, 4, 128], BF16)
        TMP = [al([128, 512], F32) for _ in range(2)]
        SM = self.SM
        P.D("pool", WUKT.re("p a b -> p (a b)"), self.wukT)
        P.D("pool", WUV, self.w_uv)
        P.Dk("sp", FB, self.CKVT, reads=["CKVT"])
        P.I("act", "activation", dict(out=F2), dict(in_=FB), func=AF.Square)
        for g in range(8):
            cs = slice(g * 512, (g + 1) * 512)
            pb = B[g % 2]
            P.mm(pb, self.ONES128, F2[:, cs])
            P.I("act", "activation", dict(out=T1), dict(in_=pb), func=AF.Sqrt, bias=EPS)
            P.I("dve", "reciprocal", dict(out=T1), dict(in_=T1))
            P.I("dve", "scalar_tensor_tensor", dict(out=KVT[:, cs]), dict(in0=FB[:, cs], scalar=SM[:, 0:1], in1=T1),
                op0=ALU.mult, op1=ALU.mult)
        for q in range(8):
            pb = B[2 + q % 2]
            for b4 in range(4):
                jb = q * 4 + b4
                P.mm(pb[:, b4 * 128:(b4 + 1) * 128], KVT[:, jb * 128:(jb + 1) * 128], self.IDB)
            P.I("act", "activation", dict(out=CKV[:, q * 4:(q + 1) * 4, :]), dict(in_=pb.re("p (a b) -> p a b", a=4)), func=AF.Copy)
        for which, dst in (("kr", KR2), ("ik", IK2)):
            if which == "kr":
                P.I("pool", "memset", dict(ap=FB), {}, constant=0.0)
                P.Dk("sp", FB[0:16, :], self.KRT, reads=["KRT"])
                P.Dk("sp", FB[64:80, :], self.KRT, reads=["KRT"])
            else:
                P.Dk("sp", FB[0:64, :], self.IKT, reads=["IKT"])
                P.Dk("sp", FB[64:128, :], self.IKT, reads=["IKT"])
            for g in range(8):
                cs = slice(g * 512, (g + 1) * 512)
                pb = B[g % 2]
                P.D("sp", TA[0], self.ropeA[0, :, cs])
                P.D("sp", TA[1], self.ropeA[1, :, cs])
                P.mm(pb, self.PROT, FB[:, cs])
                P.I("dve", "tensor_tensor", dict(out=T1), dict(in0=FB[:, cs], in1=TA[0]), op=ALU.mult)
                P.I("dve", "tensor_tensor", dict(out=T2), dict(in0=pb, in1=TA[1]), op=ALU.mult)
                P.I("pool", "tensor_tensor", dict(out=dst[:, cs]), dict(in0=T1, in1=T2), op=ALU.add)
        SC = FB
        I4 = self.I4
        tix = 0
        for m in range(self.nblk):
            S = 128 * (2 * m + 2)
            bs = slice(m * 128, (m + 1) * 128)
            P.Dk("sp", DQR, self.DQT.rearrange("(a p) t -> p a t", p=128)[:, :, bs], reads=["DQT"])
            P.Dk("sp", IQR, self.IQT.rearrange("(a p) t -> p a t", p=128)[:, :, bs], reads=["IQT"])
            P.Dk("sp", IW, self.IWS[bs, :], reads=["IWS"])
            P.Dk("sp", GZ, self.DZT.rearrange("(a p) t -> p a t", p=128)[:, :, bs], reads=["DZT"])
            P.D("sp", TA[0], self.ropeO[0, :, m, :])
            P.D("sp", TA[1], self.ropeO[1, :, m, :])
            pb = B[6]
            for a in range(4):
                P.mm(pb[:, a * 128:(a + 1) * 128], self.PROT, DQR[:, a, :])
            P.I("dve", "tensor_tensor", dict(out=T1), dict(in0=DQR.re("p a b -> p (a b)"), in1=TA[0]), op=ALU.mult)
            P.I("dve", "tensor_tensor", dict(out=T2), dict(in0=pb, in1=TA[1]), op=ALU.mult)
            P.I("pool", "tensor_tensor", dict(out=DQ.re("p a b -> p (a b)")), dict(in0=T1, in1=T2), op=ALU.add)
            dqm4 = DQM.re("p (a e) t -> p a e t", e=2)
            for e in range(2):
                P.I("dve", "tensor_scalar", dict(out=dqm4[:, :, e, :]), dict(in0=DQ, scalar1=SM[:, 44 + e:45 + e]),
                    scalar2=None, op0=ALU.mult)
            pb = B[7]
            for a in range(2):
                P.mm(pb[:, a * 128:(a + 1) * 128], self.PROT, IQR[:, a, :])
            P.I("dve", "tensor_tensor", dict(out=T1[:, 0:256]), dict(in0=IQR.re("p a b -> p (a b)"), in1=TA[0][:, 0:256]), op=ALU.mult)
            P.I("dve", "tensor_tensor", dict(out=T2[:, 0:256]), dict(in0=pb[:, 0:256], in1=TA[1][:, 0:256]), op=ALU.mult)
            P.I("pool", "tensor_tensor", dict(out=IQ.re("p a b -> p (a b)")), dict(in0=T1[:, 0:256], in1=T2[:, 0:256]), op=ALU.add)
            iqm4 = IQM.re("p (a e) t -> p a e t", e=2)
            for e in range(2):
                P.I("dve", "tensor_scalar", dict(out=iqm4[:, :, e, :]), dict(in0=IQ, scalar1=SM[:, 44 + e:45 + e]),
                    scalar2=None, op0=ALU.mult)
            for q in range(2):
                pb = B[6 + q]
                for h4 in range(4):
                    h = q * 4 + h4
                    pr, hb = h // 2, (h % 2) * 64
                    P.mm(pb[:, h4 * 128:(h4 + 1) * 128], WUKT[:, pr, :], DQM[:, h, :])
                P.I("act", "activation", dict(out=QABS[:, q * 4:(q + 1) * 4, :]), dict(in_=pb.re("p (a b) -> p a b", a=4)), func=AF.Copy)
            for k0 in range(0, S, 512):
                n = min(512, S - k0)
                for hi in range(4):
                    pr, hb = hi // 2, (hi % 2) * 64
                    pb = B[hi % 2]
                    P.mm(pb[:, 0:n], IQM[:, hi, :], IK2[:, k0:k0 + n])
                    if hi == 0:
                        P.I("dve", "tensor_scalar", dict(out=SC[:, k0:k0 + n]), dict(in0=pb[:, 0:n], scalar2=IW[:, 0:1]),
                            scalar1=0.0, op0=ALU.max, op1=ALU.mult)
                    else:
                        tm = TMP[hi % 2]
                        P.I("dve", "tensor_scalar", dict(out=tm[:, 0:n]), dict(in0=pb[:, 0:n], scalar2=IW[:, hi:hi + 1]),
                            scalar1=0.0, op0=ALU.max, op1=ALU.mult)
                        P.I("pool", "tensor_tensor", dict(out=SC[:, k0:k0 + n]), dict(in0=SC[:, k0:k0 + n], in1=tm[:, 0:n]), op=ALU.add)
            P.I("dve", "tensor_reduce", dict(out=BS[:, 0:1]), dict(in_=SC[:, 0:S]), axis=AX.X, op=ALU.max)
            P.I("dve", "tensor_reduce", dict(out=BS[:, 1:2]), dict(in_=SC[:, 0:S]), axis=AX.X, op=ALU.min)
            P.I("pool", "tensor_tensor", dict(out=SC[:, S - 256:S]), dict(in0=SC[:, S - 256:S], in1=self.MQ), op=ALU.add)
            P.I("dve", "tensor_tensor", dict(out=BS[:, 2:3]), dict(in0=BS[:, 0:1], in1=BS[:, 1:2]), op=ALU.subtract)
            for it in range(nbis):
                P.I("dve", "tensor_scalar", dict(out=BS[:, 8 + it:9 + it]), dict(in0=BS[:, 2:3]),
                    scalar1=float(0.5 ** (it + 1)), scalar2=None, op0=ALU.mult)
            for it in range(nbis):
                wk = BS[:, 8 + it:9 + it]
                P.I("dve", "tensor_tensor", dict(out=BS[:, 3:4]), dict(in0=BS[:, 1:2], in1=wk), op=ALU.add)
                P.I("dve", "tensor_scalar", dict(out=F2[:, 0:S], accum_out=BS[:, 4:5]), dict(in0=SC[:, 0:S], scalar1=BS[:, 3:4]),
                    scalar2=0.0, op0=ALU.is_ge, op1=ALU.add)
                P.I("dve", "tensor_scalar", dict(out=BS[:, 5:6]), dict(in0=BS[:, 4:5], scalar2=wk),
                    scalar1=256.0, op0=ALU.is_ge, op1=ALU.mult)
                P.I("dve", "tensor_tensor", dict(out=BS[:, 1:2]), dict(in0=BS[:, 1:2], in1=BS[:, 5:6]), op=ALU.add)
            P.I("dve", "tensor_scalar", dict(out=MA[:, 0:S]), dict(in0=SC[:, 0:S], scalar1=BS[:, 1:2]),
                scalar2=NEG, op0=ALU.is_lt, op1=ALU.mult)
            jb = slice(2 * m * 128, (2 * m + 1) * 128)
            for q in range(2):
                pb = B[6 + q]
                for h4 in range(4):
                    h = q * 4 + h4
                    pr, hb = h // 2, (h % 2) * 64
                    o = pb[:, h4 * 128:(h4 + 1) * 128]
                    P.mm(o, QABS[:, h, :], KVT[:, jb], start=True, stop=False)
                    P.mm(o, DQM[:, h, :], KR2[:, jb], start=False, stop=True)
                P.I("dve", "tensor_reduce", dict(out=MX[:, q * 4:(q + 1) * 4]), dict(in_=pb.re("p (a b) -> p a b", a=4)),
                    axis=AX.X, op=ALU.max)
            P.I("dve", "tensor_scalar", dict(out=MX), dict(in0=MX), scalar1=-1.0, scalar2=None, op0=ALU.mult)
            for h in range(8):
                P.I("dve", "tensor_scalar", dict(out=RD[:, h, :]), dict(in0=self.IDB, scalar1=MX[:, h:h + 1]), scalar2=None, op0=ALU.mult)
            nj = 2 * m + 2
            for hg in range(2):
                po, prs = B[4], B[5]
                for j in range(nj):
                    s = tix % 2
                    tix += 1
                    pl = B[s]
                    js = slice(j * 128, (j + 1) * 128)
                    for hh in range(4):
                        h = 4 * hg + hh
                        pr, hb = h // 2, (h % 2) * 64
                        o = pl[:, hh * 128:(hh + 1) * 128]
                        P.mm(o, KVT[:, js], QABS[:, h, :], start=(hh == 0), stop=False)
                        P.mm(o, KR2[:, js], DQM[:, h, :], start=False, stop=False)
                    P.mm(pl, MA[:, js], I4, start=False, stop=False)
                    P.mm(pl, self.ONESB, RD[:, hg * 4:(hg + 1) * 4, :].re("p a b -> p (a b)"), start=False, stop=True)
                    P.I("act", "activation", dict(out=PTt[s]), dict(in_=pl), func=AF.Exp)
                    P.mm(po, CKV[:, j, :], PTt[s], start=(j == 0), stop=(j == nj - 1))
                    P.mm(prs, self.ONESB, PTt[s], start=(j == 0), stop=(j == nj - 1))
                P.I("dve", "reciprocal", dict(out=RS), dict(in_=prs))
                P.I("dve", "tensor_tensor", dict(out=OLN.re("p a b -> p (a b)")), dict(in0=po, in1=RS), op=ALU.mult)
                py = B[6 + hg]
                for hh in range(4):
                    h = 4 * hg + hh
                    pr, hb = h // 2, (h % 2) * 64
                    P.mm(py[hb:hb + 64, (pr - 2 * hg) * 128:(pr - 2 * hg + 1) * 128], WUV[:, h * 64:(h + 1) * 64], OLN[:, hh, :])
                P.I("dve", "tensor_tensor", dict(out=YB[:, 2 * hg:2 * hg + 2, :]),
                    dict(in0=py[:, 0:256].re("p (a b) -> p a b", a=2), in1=GZ[:, 2 * hg:2 * hg + 2, :]), op=ALU.mult)
            P.Dk("sp", self.BR[0].rearrange("(a p) t -> p a t", p=128)[:, :, bs], YB, writes=["BR"])

    def phase_tail(self):
        P, nc, B = self.P, self.nc, self.B
        P.barrier()
        al = Alloc(nc, PH0, "tl")
        BRt = al([128, 12, 2048], BF16)
        MXT = al([128, 8, 2048], BF16)
        WO = al([128, 8, 1024], BF16)
        WBd = [al([128, 12, 128], BF16) for _ in range(2)]
        SG = [[al([128, 512], BF16) for _ in range(3)] for _ in range(2)]
        M0 = [al([128, 512], F32) for _ in range(2)]
        M1 = [al([128, 512], F32) for _ in range(2)]
        M2 = [al([128, 512], F32) for _ in range(2)]
        XT = [al([128, 1024], F32) for _ in range(2)]
        XN = [al([128, 1024], F32) for _ in range(2)]
        JUNK = al([128, 1024], BF16)
        SS = al([128, 8], F32)
        brv = self.BR.rearrange("g (c p) t -> p (g c) t", p=128)
        for a in range(12):
            P.Dk("sp", BRt[:, a, :], brv[:, a, :], reads=["BR"])
        P.D("pool", WO, self.w_out.rearrange("(k p) n -> p k n", p=128))
        if self.last:
            P.D("sp", self.NWB, self.fnwb)
        wbv = self.w_branch.rearrange("g (c p) n -> p (g c) n", p=128)
        it = 0
        for dt in range(8):
            wb = WBd[dt % 2]
            P.D("pool", wb, wbv[:, :, dt * 128:(dt + 1) * 128])
            for grp in range(4):
                s = it % 2
                it += 1
                gs = slice(grp * 512, (grp + 1) * 512)
                for g in range(3):
                    P.Dk("sp", SG[s][g], self.MRG[g * 1024 + dt * 128:g * 1024 + (dt + 1) * 128, gs], reads=["MRG"])
                    pb = B[s * 3 + g]
                    for ct in range(4):
                        P.mm(pb, wb[:, g * 4 + ct, :], BRt[:, g * 4 + ct, gs], start=(ct == 0), stop=(ct == 3))
                P.I("dve", "tensor_tensor", dict(out=M0[s]), dict(in0=B[s * 3 + 0], in1=SG[s][0]), op=ALU.mult)
                P.I("dve", "tensor_tensor", dict(out=M1[s]), dict(in0=B[s * 3 + 1], in1=SG[s][1]), op=ALU.mult)
                P.I("dve", "tensor_tensor", dict(out=M2[s]), dict(in0=B[s * 3 + 2], in1=SG[s][2]), op=ALU.mult)
                P.I("pool", "tensor_tensor", dict(out=M0[s]), dict(in0=M0[s], in1=M1[s]), op=ALU.add)
                P.I("pool", "tensor_tensor", dict(out=MXT[:, dt, gs]), dict(in0=M0[s], in1=M2[s]), op=ALU.add)
        for blk in range(16):
            s = blk % 2
            bs = slice(blk * 128, (blk + 1) * 128)
            P.D("sp", XT[s], self.x_own[bs, :])
            for half in range(2):
                pb = B[6 + half]
                hs = slice(half * 512, (half + 1) * 512)
                for dt in range(8):
                    P.mm(pb, MXT[:, dt, bs], WO[:, dt, hs], start=(dt == 0), stop=(dt == 7))
                P.I("dve", "tensor_tensor", dict(out=XN[s][:, hs]), dict(in0=pb, in1=XT[s][:, hs]), op=ALU.add)
            if self.last:
                P.I("act", "activation", dict(out=JUNK, accum_out=SS[:, 0:1]), dict(in_=XN[s]), func=AF.Square)
                P.I("act", "activation", dict(out=SS[:, 1:2]), dict(in_=SS[:, 0:1]), func=AF.Sqrt, scale=1.0 / 1024, bias=EPS)
                P.I("dve", "reciprocal", dict(out=SS[:, 2:3]), dict(in_=SS[:, 1:2]))
                P.I("dve", "scalar_tensor_tensor", dict(out=XN[s]), dict(in0=XN[s], scalar=SS[:, 2:3], in1=self.NWB),
                    op0=ALU.mult, op1=ALU.mult)
            self.out_evs.append(P.Dk("sp", self.out[bs, :], XN[s]))

    def build(self, stack):
        self.out_evs = []
        for ph in self.phases:
            dict(A=self.pass_A, B=self.pass_B, sb=self.phase_sb, ml=self.phase_ml,
                 dsa=self.phase_dsa, tail=self.phase_tail)[ph]()
        self.P.barrier()
        self.P.emit(stack)
        return self.nc


def _consts():
    f = np.float32
    ident = np.eye(128, dtype=f)
    prot = np.zeros((128, 128), f)
    for p in range(128):
        d = p % 64
        if d < 8:
            prot[p + 8, p] = -1.0
        elif d < 16:
            prot[p - 8, p] = 1.0
    cf = np.concatenate([ident, prot, np.ones((128, 128), f), np.full((128, 128), 1.0 / 128, f)], 1)
    negtri = np.zeros((128, 128), f)
    for sp in range(128):
        negtri[sp, :sp + 1] = -1.0
    cb = np.concatenate([ident, np.ones((128, 128), f), -np.ones((128, 128), f), negtri, np.tile(ident, (1, 4))], 1)
    sel = np.zeros((4, 4, 128), f)
    for h in range(4):
        sel[h, h, :] = 1.0
    sel = sel.reshape(4, 512)
    pos = np.arange(4096, dtype=np.float32)
    inv = (np.float32(500000.0) ** (-np.arange(0, 16, 2, dtype=np.float32) / np.float32(16))).astype(np.float32)
    ang = pos[:, None] * inv[None, :]
    cos, sin = np.cos(ang).astype(f), np.sin(ang).astype(f)
    ca = np.ones((128, 4096), f)
    sa = np.zeros((128, 4096), f)
    for p in range(128):
        d = p % 64
        if d < 16:
            ca[p] = cos[:, d % 8]
            sa[p] = sin[:, d % 8]
    return cf, cb, sel, ca, sa


def _core_consts(c, ca, sa):
    f = np.float32
    s = np.arange(128)[:, None]
    t = np.arange(128)[None, :]
    strict = (s < t).astype(f)
    incl = (s <= t).astype(f)
    one = np.ones((128, 128), f)
    zero = np.zeros((128, 128), f)
    if c == 0:
        sb_mul, mi_mul = [strict, zero], [incl, zero]
    else:
        sb_mul, mi_mul = [one, strict], [one, incl]
    sb_add = [(1.0 - a) * NEG for a in sb_mul]
    rep = lambda lst: np.concatenate([np.tile(a, (1, 4)) for a in lst], 1)
    msk = np.concatenate([rep(sb_mul), rep(sb_add), rep(mi_mul)], 1).astype(f)
    qm = [(1.0 - a.T) * -1e30 for a in mi_mul]
    mq = np.concatenate(qm, 1).astype(f)
    own = np.arange(16) * 2 + c
    ro = np.zeros((2, 128, 16, 512), f)
    for m, i in enumerate(own):
        ro[0, :, m, :] = np.tile(ca[:, i * 128:(i + 1) * 128], (1, 4)) * 0.125
        ro[1, :, m, :] = np.tile(sa[:, i * 128:(i + 1) * 128], (1, 4)) * 0.125
    return msk, mq, ro


_NC_CACHE = {}


def _get_nc(last):
    if last not in _NC_CACHE:
        from contextlib import ExitStack
        L = Layer(last)
        with ExitStack() as st:
            L.build(st)
        _NC_CACHE[last] = L.nc
    return _NC_CACHE[last]


def _layer_inputs(l, x, w_in, w_dsa_uk, w_dsa_uv, dsa_kv_norm_w, ml_conv_w, ml_i_bias, ml_f_bias,
                  ml_norm_w, w_branch, w_out, norm_w, final_norm_w, shared):
    f = np.float32
    cf, cb, sel, ca, sa, core = shared
    wuk = np.zeros((128, 8, 64), f)
    wuk[:, :, 16:] = w_dsa_uk[l]
    wukT = np.ascontiguousarray(wuk.reshape(128, 4, 2, 64).transpose(2, 3, 1, 0).reshape(128, 512))
    small_base = np.zeros((128, 64), f)
    small_base[:, 0] = dsa_kv_norm_w[l]
    cw = ml_conv_w[l]
    small_base[:, 1:33] = cw.reshape(4, 8, 128).transpose(2, 1, 0).reshape(128, 32)
    small_base[0:4, 33] = ml_i_bias[l]
    small_base[0:4, 34] = ml_f_bias[l]
    small_base[:, 36:40] = ml_norm_w[l].reshape(4, 128).T
    maps = []
    for b in range(4):
        for c in range(2):
            msk, mq, ro = core[c]
            sm = small_base.copy()
            sm[:, 40] = 1.0 - c
            sm[:, 41] = float(c)
            sm[0:64, 44] = 1.0
            sm[64:128, 45] = 1.0
            xo = x[b].reshape(16, 2, 128, 1024)[:, c].reshape(2048, 1024)
            maps.append(dict(
                x_all=np.ascontiguousarray(x[b]), x_own=np.ascontiguousarray(xo), w_in=w_in[l],
                wukT=wukT, w_uv=np.ascontiguousarray(w_dsa_uv[l].reshape(128, 512)), w_branch=w_branch[l],
                w_out=w_out[l], nwb=np.ascontiguousarray(np.broadcast_to(norm_w[l][None, :], (128, 1024))),
                fnwb=np.ascontiguousarray(np.broadcast_to(final_norm_w[None, :], (128, 1024))),
                small=sm, cf=cf, cb=cb, msk=msk, mq=mq, sel=sel,
                ropeA=np.stack([ca, sa]), ropeO=ro))
    return maps


def kernel(x, w_in, w_dsa_uk, w_dsa_uv, dsa_kv_norm_w, ml_conv_w, ml_i_bias, ml_f_bias,
           ml_norm_w, w_branch, w_out, norm_w, final_norm_w):
    args = [np.asarray(a, dtype=np.float32) for a in
            (x, w_in, w_dsa_uk, w_dsa_uv, dsa_kv_norm_w, ml_conv_w, ml_i_bias, ml_f_bias,
             ml_norm_w, w_branch, w_out, norm_w, final_norm_w)]
    x = args[0]
    cf, cb, sel, ca, sa = _consts()
    core = [_core_consts(c, ca, sa) for c in range(2)]
    shared = (cf, cb, sel, ca, sa, core)
    depth = args[1].shape[0]
    for l in range(depth):
        nc = _get_nc(l == depth - 1)
        maps = _layer_inputs(l, x, *args[1:], shared)
        res = run_bass_kernel_spmd(nc, maps, core_ids=list(range(8)))
        xn = np.empty_like(x)
        for b in range(4):
            for c in range(2):
                o = res.results[b * 2 + c]["out"]
                xn[b].reshape(16, 2, 128, 1024)[:, c] = o.reshape(16, 128, 1024)
        x = xn
    return x
```
